# Optimizing a Trainium2 kernel written in Bass

```python
import math
import jax
import jax.numpy as jnp
from jax import lax
import numpy as np

D_MODEL = 2048
BATCH = 4
SEQ = 2048
DEPTH = 2

N_HEADS = 8
HEAD_DIM = D_MODEL // N_HEADS // 2
Q_BLOCK = 128
N_BUCKETS = 32
MAX_DISTANCE = 128
RG_WIDTH = D_MODEL
RG_BLOCKS = 8
RG_BW = RG_WIDTH // RG_BLOCKS
RG_C = 8.0
CONV_W = 4
N_EXPERTS = 32
TOP_K = 4
D_FF = D_MODEL
SWIGLU_LIMIT = 7.0
SWIGLU_ALPHA = 1.702
EXPERT_BLOCK = 256
EPS = 1e-6

kernel_name = "hybrid_diffattn_rglru_moe_adaln"


def rms_norm(x, g):
    xf = x.astype(jnp.float32)
    y = xf * lax.rsqrt(jnp.mean(xf * xf, axis=-1, keepdims=True) + EPS)
    return (y * g.astype(jnp.float32)).astype(x.dtype)


def t5_bucket(rel):
    nb = N_BUCKETS // 2
    max_exact = nb // 2
    ret = jnp.where(rel > 0, nb, 0)
    n = jnp.abs(rel)
    nf = jnp.maximum(n, 1).astype(jnp.float32)
    large = max_exact + (jnp.log(nf / max_exact) / math.log(MAX_DISTANCE / max_exact)
                         * (nb - max_exact)).astype(jnp.int32)
    large = jnp.minimum(large, nb - 1)
    return ret + jnp.where(n < max_exact, n, large)


def diff_attention(h, rel_table, w_in, q_gain, k_gain, lq1, lk1, lq2, lk2, subln_g, w_out, lambda_init):
    B, S, D = h.shape
    qkv = h @ w_in
    q, k, v = jnp.split(qkv, 3, axis=-1)
    q = rms_norm(q.reshape(B, S, 2 * N_HEADS, HEAD_DIM), q_gain) * (HEAD_DIM ** -0.5)
    k = rms_norm(k.reshape(B, S, 2 * N_HEADS, HEAD_DIM), k_gain)
    v = v.reshape(B, S, N_HEADS, 2 * HEAD_DIM)
    lam = (jnp.exp(jnp.sum(lq1.astype(jnp.float32) * lk1.astype(jnp.float32)))
           - jnp.exp(jnp.sum(lq2.astype(jnp.float32) * lk2.astype(jnp.float32))) + lambda_init)
    n_q = S // Q_BLOCK
    q_blocks = q.reshape(B, n_q, Q_BLOCK, 2 * N_HEADS, HEAD_DIM).transpose(1, 0, 2, 3, 4)
    starts = jnp.arange(n_q, dtype=jnp.int32) * Q_BLOCK
    k_pos = jnp.arange(S, dtype=jnp.int32)

    def block(args):
        qi, start = args
        s = jnp.einsum("bqhd,bkhd->bhqk", qi, k).astype(jnp.float32)
        s = s.reshape(B, N_HEADS, 2, Q_BLOCK, S)
        q_pos = start + jnp.arange(Q_BLOCK, dtype=jnp.int32)
        bias = rel_table[t5_bucket(k_pos[None, :] - q_pos[:, None])]
        s = s + jnp.transpose(bias, (2, 0, 1)).astype(jnp.float32)[None, :, None]
        p = jax.nn.softmax(s, axis=-1)
        attn = p[:, :, 0] - lam * p[:, :, 1]
        return jnp.einsum("bhqk,bkhe->bqhe", attn.astype(v.dtype), v)

    o = lax.map(block, (q_blocks, starts))
    o = o.transpose(1, 0, 2, 3, 4).reshape(B, S, N_HEADS, 2 * HEAD_DIM)
    o = rms_norm(o, subln_g) * (1.0 - lambda_init)
    return o.reshape(B, S, D) @ w_out


def _linear_combine(c1, c2):
    a1, b1 = c1
    a2, b2 = c2
    return a1 * a2, a2 * b1 + b2


def rg_lru(u, w_a, b_a, w_x, b_x, lam, reverse):
    B, S, W = u.shape
    ug = u.reshape(B, S, RG_BLOCKS, RG_BW)
    r = jax.nn.sigmoid((jnp.einsum("bsnc,ncd->bsnd", ug, w_a).reshape(B, S, W) + b_a).astype(jnp.float32))
    i = jax.nn.sigmoid((jnp.einsum("bsnc,ncd->bsnd", ug, w_x).reshape(B, S, W) + b_x).astype(jnp.float32))
    log_a = -RG_C * r * jax.nn.softplus(-lam.astype(jnp.float32))
    a = jnp.exp(log_a)
    b = jnp.sqrt(-jnp.expm1(2.0 * log_a)) * (i * u.astype(jnp.float32))
    _, hs = lax.associative_scan(_linear_combine, (a, b), axis=1, reverse=reverse)
    return hs


def recurrent_block(h, w_in, conv_w, conv_b, wa, ba, wx, bx, lam, w_out):
    B, S, D = h.shape
    proj = h @ w_in
    gate_br, rec_br = jnp.split(proj, 2, axis=-1)
    gate_br = jax.nn.gelu(gate_br)
    u = lax.conv_general_dilated(rec_br, conv_w[:, None, :].astype(rec_br.dtype), window_strides=(1,),
                                 padding=[(2, 1)], dimension_numbers=("NWC", "WIO", "NWC"),
                                 feature_group_count=RG_WIDTH) + conv_b
    hf = rg_lru(u, wa[0], ba[0], wx[0], bx[0], lam[0], reverse=False)
    hb = rg_lru(u, wa[1], ba[1], wx[1], bx[1], lam[1], reverse=True)
    y = (hf + hb).astype(h.dtype) * gate_br
    return y @ w_out


def moe(xn, router_w, router_b, w_gu, b_gu, w_down, b_down):
    T, D = xn.shape
    logits = (xn @ router_w + router_b).astype(jnp.float32)
    top_val, top_idx = lax.top_k(logits, TOP_K)
    gates = jax.nn.softmax(top_val, axis=-1)
    TK = T * TOP_K
    flat_e = top_idx.reshape(-1).astype(jnp.int32)
    flat_tok = jnp.repeat(jnp.arange(T, dtype=jnp.int32), TOP_K)
    flat_g = gates.reshape(-1)
    order = jnp.argsort(flat_e, stable=True)
    se, stok, sg = flat_e[order], flat_tok[order], flat_g[order]
    counts = jnp.bincount(flat_e, length=N_EXPERTS)
    padded = (counts + EXPERT_BLOCK - 1) // EXPERT_BLOCK * EXPERT_BLOCK
    pad_end = jnp.cumsum(padded)
    pad_start = pad_end - padded
    start = jnp.cumsum(counts) - counts
    dest = pad_start[se] + (jnp.arange(TK, dtype=jnp.int32) - start[se])
    n_blocks = -(-TK // EXPERT_BLOCK) + N_EXPERTS
    cap = n_blocks * EXPERT_BLOCK
    slot_tok = jnp.zeros((cap,), jnp.int32).at[dest].set(stok)
    slot_g = jnp.zeros((cap,), jnp.float32).at[dest].set(sg)
    block_starts = jnp.arange(n_blocks, dtype=jnp.int32) * EXPERT_BLOCK
    block_e = jnp.clip(jnp.searchsorted(pad_end, block_starts, side="right"), 0, N_EXPERTS - 1)

    def expert_block(args):
        tok, e = args
        xb = xn[tok]
        gu = xb @ w_gu[e] + b_gu[e]
        g, u = jnp.split(gu, 2, axis=-1)
        g = jnp.minimum(g, SWIGLU_LIMIT)
        u = jnp.clip(u, -SWIGLU_LIMIT, SWIGLU_LIMIT)
        hdn = (u + 1.0) * (g * jax.nn.sigmoid(SWIGLU_ALPHA * g))
        return hdn @ w_down[e] + b_down[e]

    out = lax.map(expert_block, (slot_tok.reshape(n_blocks, EXPERT_BLOCK), block_e))
    out = out.reshape(cap, D)
    return jnp.zeros_like(xn).at[slot_tok].add(out * slot_g[:, None].astype(out.dtype))


def setup_inputs(seed: int = 0) -> dict:
    key = jax.random.key(seed)
    ks = iter(jax.random.split(key, 40))
    f32 = jnp.float32

    def nrm(shape, std):
        return jax.random.normal(next(ks), shape, f32) * std

    D, W, E, F = D_MODEL, RG_WIDTH, N_EXPERTS, D_FF
    n_attn = (DEPTH + 1) // 2
    n_rec = DEPTH // 2
    u = jax.random.uniform(next(ks), (n_rec, 2, W), f32, minval=0.9, maxval=0.999)
    s = u ** (1.0 / RG_C)
    rec_lam = jnp.log(s) - jnp.log1p(-s)
    return {
        "x": nrm((BATCH, SEQ, D), 1.0),
        "c": nrm((BATCH, D), 1.0),
        "rel_table": nrm((N_BUCKETS, N_HEADS), 0.5),
        "mod_w": nrm((DEPTH, D, 6 * D), 0.3 * D ** -0.5),
        "mod_b": nrm((DEPTH, 6 * D), 0.02),
        "norm1_g": 1.0 + nrm((DEPTH, D), 0.02),
        "norm2_g": 1.0 + nrm((DEPTH, D), 0.02),
        "attn_w_in": nrm((n_attn, D, 3 * D), D ** -0.5),
        "attn_q_gain": 1.0 + nrm((n_attn, HEAD_DIM), 0.02),
        "attn_k_gain": 1.0 + nrm((n_attn, HEAD_DIM), 0.02),
        "attn_lq1": nrm((n_attn, HEAD_DIM), 0.1),
        "attn_lk1": nrm((n_attn, HEAD_DIM), 0.1),
        "attn_lq2": nrm((n_attn, HEAD_DIM), 0.1),
        "attn_lk2": nrm((n_attn, HEAD_DIM), 0.1),
        "attn_subln_g": 1.0 + nrm((n_attn, 2 * HEAD_DIM), 0.02),
        "attn_w_out": nrm((n_attn, D, D), D ** -0.5),
        "rec_w_in": nrm((n_rec, D, 2 * W), D ** -0.5),
        "rec_conv_w": nrm((n_rec, CONV_W, W), CONV_W ** -0.5),
        "rec_conv_b": nrm((n_rec, W), 0.02),
        "rec_wa": nrm((n_rec, 2, RG_BLOCKS, RG_BW, RG_BW), RG_BW ** -0.5),
        "rec_ba": nrm((n_rec, 2, W), 0.02),
        "rec_wx": nrm((n_rec, 2, RG_BLOCKS, RG_BW, RG_BW), RG_BW ** -0.5),
        "rec_bx": nrm((n_rec, 2, W), 0.02),
        "rec_lam": rec_lam,
        "rec_w_out": nrm((n_rec, W, D), W ** -0.5),
        "moe_router_w": nrm((DEPTH, D, E), D ** -0.5),
        "moe_router_b": nrm((DEPTH, E), 0.01),
        "moe_w_gu": nrm((DEPTH, E, D, 2 * F), D ** -0.5),
        "moe_b_gu": nrm((DEPTH, E, 2 * F), 0.02),
        "moe_w_down": nrm((DEPTH, E, F, D), F ** -0.5),
        "moe_b_down": nrm((DEPTH, E, D), 0.02),
    }


def reference(x, c, rel_table, mod_w, mod_b, norm1_g, norm2_g,
              attn_w_in, attn_q_gain, attn_k_gain, attn_lq1, attn_lk1, attn_lq2, attn_lk2,
              attn_subln_g, attn_w_out,
              rec_w_in, rec_conv_w, rec_conv_b, rec_wa, rec_ba, rec_wx, rec_bx, rec_lam, rec_w_out,
              moe_router_w, moe_router_b, moe_w_gu, moe_b_gu, moe_w_down, moe_b_down):
    B, S, D = x.shape
    for l in range(DEPTH):
        mod = c @ mod_w[l] + mod_b[l]
        sh1, sc1, g1, sh2, sc2, g2 = [m[:, None, :] for m in jnp.split(mod, 6, axis=-1)]
        h = rms_norm(x, norm1_g[l]) * (1.0 + sc1) + sh1
        j = l // 2
        if l % 2 == 0:
            lambda_init = 0.8 - 0.6 * math.exp(-0.3 * l)
            y = diff_attention(h, rel_table, attn_w_in[j], attn_q_gain[j], attn_k_gain[j],
                               attn_lq1[j], attn_lk1[j], attn_lq2[j], attn_lk2[j],
                               attn_subln_g[j], attn_w_out[j], lambda_init)
        else:
            y = recurrent_block(h, rec_w_in[j], rec_conv_w[j], rec_conv_b[j], rec_wa[j], rec_ba[j],
                                rec_wx[j], rec_bx[j], rec_lam[j], rec_w_out[j])
        x = x + g1 * y
        h = rms_norm(x, norm2_g[l]) * (1.0 + sc2) + sh2
        f = moe(h.reshape(B * S, D), moe_router_w[l], moe_router_b[l], moe_w_gu[l], moe_b_gu[l],
                moe_w_down[l], moe_b_down[l])
        x = x + g2 * f.reshape(B, S, D)
    return x
```

```python
import math
import numpy as np
import concourse.bass as bass
import concourse.mybir as mybir
from concourse.bass_utils import run_bass_kernel_spmd

F32 = mybir.dt.float32
BF16 = mybir.dt.bfloat16
ALU = mybir.AluOpType
AF = mybir.ActivationFunctionType
AX = mybir.AxisListType

D = 2048
S = 2048
OWN = 1024
NT = 8
NTF = 16
DC = 16
NE = 32
CAP = 512
NCH = CAP // 128
EPS = 1e-6
ARENA = 206 * 1024


class Buf:
    __slots__ = ("name", "w", "r")

    def __init__(self, name):
        self.name = name
        self.w = None
        self.r = {}


class KB:
    ENG = ("pe", "act", "dve", "pool", "sp")
    NDMA = 12

    def __init__(self, nc):
        self.nc = nc
        self.q = {e: [] for e in self.ENG}
        self.cnt = {e: 0 for e in self.ENG}
        self.known = {e: {} for e in self.ENG}
        self.sem = {}
        for e in self.ENG:
            self.sem["E" + e] = nc.alloc_semaphore(name="E" + e)
        self.dma_i = {}
        self.dma_val = {}
        for qn in ("sp", "pool", "act"):
            self.dma_i[qn] = 0
            self.dma_val[qn] = [0] * self.NDMA
            for i in range(self.NDMA):
                self.sem["D%s%d" % (qn, i)] = nc.alloc_semaphore(name="D%s%d" % (qn, i))
        self.ninstr = 0
        self.bufs = {}

    def B(self, name):
        b = self.bufs.get(name)
        if b is None:
            b = Buf(name)
            self.bufs[name] = b
        return b

    def _bl(self, lst):
        return [self.B(x) if isinstance(x, str) else x for x in lst]

    def _need(self, eng, deps):
        kn = self.known[eng]
        for (s, v) in deps:
            if s == "Epe" and eng == "pe":
                continue
            if kn.get(s, 0) < v:
                kn[s] = v
                self.q[eng].append(("w", s, v))
                self.ninstr += 1

    def _deps(self, R, W):
        deps = []
        for b in R:
            if b.w is not None:
                deps.append(b.w)
        for b in W:
            if b.w is not None:
                deps.append(b.w)
            for s, v in b.r.items():
                deps.append((s, v))
        return deps

    def _mark(self, tok, R, W):
        s, v = tok
        for b in R:
            if b.r.get(s, 0) < v:
                b.r[s] = v
        for b in W:
            b.w = tok
            b.r = {}

    def op(self, eng, fn, R=(), W=(), inc=True):
        R = self._bl(R)
        W = self._bl(W)
        self._need(eng, self._deps(R, W))
        s = "E" + eng
        tok = (s, self.cnt[eng] + 1)
        if inc:
            self.cnt[eng] += 1
        self.q[eng].append(("o", fn, s if inc else None))
        self.ninstr += 1
        self._mark(tok, R, W)

    def dma(self, qn, out, in_, R=(), W=(), **kw):
        R = self._bl(R)
        W = self._bl(W)
        i = self.dma_i[qn] % self.NDMA
        self.dma_i[qn] += 1
        s = "D%s%d" % (qn, i)
        prev = self.dma_val[qn][i]
        deps = self._deps(R, W)
        if prev:
            deps.append((s, prev))
        self._need(qn, deps)
        val = prev + 16
        self.dma_val[qn][i] = val
        self.q[qn].append(("d", (lambda e: e.dma_start(out=out, in_=in_, **kw)), s))
        self.ninstr += 1
        self._mark((s, val), R, W)

    def coll(self, kind, ins, outs, groups, R=(), W=(), op=None):
        R = self._bl(R)
        W = self._bl(W)
        if "CC" not in self.sem:
            self.sem["CC"] = self.nc.alloc_semaphore(name="CCsem")
            self.cc_val = 0
        self._need("pool", self._deps(R, W))
        self.cc_val += 1
        self.q["pool"].append(("c", (lambda e: e.collective_compute(kind, (op or ALU.bypass), replica_groups=groups,
                                                                  ins=ins, outs=outs)), "CC"))
        self.ninstr += 1
        self._mark(("CC", self.cc_val), R, W)

    def barrier(self):
        deps = [("E" + e, self.cnt[e]) for e in self.ENG if self.cnt[e]]
        for qn in self.dma_val:
            for i, v in enumerate(self.dma_val[qn]):
                if v:
                    deps.append(("D%s%d" % (qn, i), v))
        if "CC" in self.sem:
            deps.append(("CC", self.cc_val))
        for e in self.ENG:
            self._need(e, [d for d in deps if d[0] != "E" + e])
        for b in self.bufs.values():
            b.w = None
            b.r = {}

    def emit(self):
        nc = self.nc
        with nc.Block() as block:
            def run(name):
                def f(e):
                    for it in self.q[name]:
                        if it[0] == "w":
                            e.wait_ge(self.sem[it[1]], it[2])
                        elif it[0] == "o":
                            ins = it[1](e)
                            if it[2] is not None:
                                ins.then_inc(self.sem[it[2]], 1)
                        elif it[0] == "c":
                            it[1](e).then_inc(self.sem[it[2]], 1)
                        else:
                            it[1](e).then_inc(self.sem[it[2]], 16)
                return f
            block.tensor(run("pe"))
            block.scalar(run("act"))
            block.vector(run("dve"))
            block.gpsimd(run("pool"))
            block.sync(run("sp"))


class Prog:
    def __init__(self, layers, dumps=()):
        self.layers = layers
        self.dumps = dumps
        nc = self.nc = bass.Bass("TRN2", target_bir_lowering=False)
        self.kb = KB(nc)
        self.arena = nc.alloc_sbuf_tensor("arena", [128, ARENA // 2], BF16)
        self.ps = nc.alloc_psum_tensor("ps", [128, 8, 512], F32)
        self.dr = {}
        self.top = 0
        self.psrr = {}

    def din(self, name, shape):
        t = self.nc.dram_tensor(name, list(shape), F32, kind="ExternalInput").ap()
        self.dr[name] = t
        return t

    def dout(self, name, shape):
        t = self.nc.dram_tensor(name, list(shape), F32, kind="ExternalOutput").ap()
        self.dr[name] = t
        return t

    def alloc(self, name, free_shape, dt):
        n = int(np.prod(free_shape))
        esz = 4 if dt == F32 else 2
        nb = (n * esz + 63) // 64 * 64
        off = self.top
        self.top += nb
        assert self.top <= ARENA, (name, self.top)
        ap = self.arena[:, off // 2: off // 2 + (n * esz) // 2]
        if dt != BF16:
            ap = ap.bitcast(dt)
        if len(free_shape) == 2:
            ap = ap.rearrange("p (a b) -> p a b", b=free_shape[1])
        elif len(free_shape) == 3:
            ap = ap.rearrange("p (a b c) -> p a b c", b=free_shape[1], c=free_shape[2])
        return ap

    def psb(self, bank, dt=F32):
        ap = self.ps[:, bank, :]
        if dt != F32:
            ap = ap.bitcast(dt)
        return ap

    def PB(self, bank):
        return "psum%d" % bank

    def mm(self, out, lhsT, rhs, start, stop, R, W, inc=True):
        self.kb.op("pe", lambda e: e.matmul(out, lhsT, rhs, start=start, stop=stop), R, W, inc)

    def tr(self, out, in_, ident, R, W, inc=True):
        self.kb.op("pe", lambda e: e.transpose(out, in_, ident), R, W, inc)

    def act(self, out, in_, func, R, W, bias=None, scale=1.0, accum=None):
        def f(e):
            kw = {}
            if bias is not None:
                kw["bias"] = bias
            if accum is not None:
                kw["accum_out"] = accum
            return e.activation(out=out, in_=in_, func=func, scale=scale, **kw)
        self.kb.op("act", f, R, W)

    def ts(self, eng, out, in0, s1, s2, op0, op1, R, W):
        en = {"dve": "vector", "pool": "gpsimd"}[eng]
        if s2 is None:
            self.kb.op(eng, lambda e: e.tensor_scalar(out, in0, s1, None, op0), R, W)
        else:
            self.kb.op(eng, lambda e: e.tensor_scalar(out, in0, s1, s2, op0, op1), R, W)

    def tt(self, eng, out, in0, in1, op, R, W):
        self.kb.op(eng, lambda e: e.tensor_tensor(out, in0, in1, op), R, W)

    def stt(self, out, in0, scalar, in1, op0, op1, R, W):
        self.kb.op("dve", lambda e: e.scalar_tensor_tensor(out, in0, scalar, in1, op0, op1), R, W)

    def cp(self, eng, out, in_, R, W):
        if eng == "act":
            self.act(out, in_, AF.Copy, R, W)
        else:
            self.kb.op(eng, lambda e: e.tensor_copy(out, in_), R, W)

    def recip(self, out, in_, R, W):
        self.kb.op("dve", lambda e: e.reciprocal(out, in_), R, W)

    def dump(self, name, ap, shape, R):
        if name not in self.dumps:
            return
        t = self.dout("dbg_" + name, shape)
        self.kb.dma("sp", t, ap, R=R, W=["dbgout"])

    def consts(self):
        kb = self.kb
        self.din("ident", [128, 128])
        self.din("tri", [128, 128])
        self.din("jrev", [128, 128])
        self.din("iotarow", [128, CAP])
        self.din("iotap", [128, NCH])
        self.din("cbb", [128, DC * 128])
        self.top = 0
        XR = self.alloc("XR", [NT * D], F32)
        self.X = XR.rearrange("p (t d) -> p t d", d=D)
        self.identf = self.alloc("identf", [128], F32)
        self.identb = self.alloc("identb", [128], BF16)
        self.jrevf = self.alloc("jrevf", [128], F32)
        self.onesb = self.alloc("onesb", [128], BF16)
        self.trib = self.alloc("trib", [128], BF16)
        self.iotarow = self.alloc("iotarow", [CAP], F32)
        self.iotap = self.alloc("iotap", [NCH], F32)
        self.cbbT = self.alloc("cbbT", [DC, 128], BF16)
        kb.dma("sp", self.identf, self.dr["ident"], W=["c_identf"])
        kb.dma("pool", self.identb, self.dr["ident"], W=["c_identb"])
        kb.dma("sp", self.jrevf, self.dr["jrev"], W=["c_jrevf"])
        kb.dma("pool", self.trib, self.dr["tri"], W=["c_trib"])
        kb.dma("sp", self.iotarow, self.dr["iotarow"], W=["c_iotarow"])
        kb.dma("sp", self.iotap, self.dr["iotap"], W=["c_iotap"])
        kb.dma("pool", self.cbbT, self.dr["cbb"].rearrange("p (k m) -> p k m", m=128), W=["c_cbbT"])
        kb.op("pool", lambda e: e.memset(self.onesb, 1.0), W=["c_onesb"])
        self.CONST_TOP = self.top
        self.MOE_BASE = self.top

    def mod_tiles(self, l, s, A, sh, g, normg, scratch, nbuf=2):
        kb = self.kb
        mark = self.top
        self.top = scratch
        modw = self.dr["mod_w%d" % l]
        modb = self.dr["mod_b%d" % l]
        wm = [self.alloc("wm%d" % i, [DC, 512], BF16) for i in range(nbuf)]
        mb = [self.alloc("mb%d" % i, [512], F32) for i in range(2)]
        ngc = [self.alloc("ngc%d" % i, [512], F32) for i in range(2)]
        tmp = self.alloc("modtmp", [512], F32)
        it = 0
        for part, dst in ((0, sh), (1, A), (2, g)):
            if dst is None:
                continue
            for n in range(4):
                base = (3 * s + part) * D + n * 512
                w = wm[it % nbuf]
                wn = "wm%d" % (it % nbuf)
                mbt = mb[it % 2]
                mbn = "mb%d" % (it % 2)
                bank = it % 2
                kb.dma("pool", w, modw[:, base:base + 512].rearrange("(k p) n -> p k n", p=128), W=[wn])
                kb.dma("sp", mbt, modb[base:base + 512].partition_broadcast(128), W=[mbn])
                for k in range(DC):
                    self.mm(self.psb(bank), self.cbbT[:, k, :], w[:, k, :], k == 0, k == DC - 1,
                            R=[wn, "c_cbbT"], W=[self.PB(bank)], inc=(k == DC - 1))
                o = dst[:, n * 512:(n + 1) * 512]
                if part == 1:
                    ngt = ngc[it % 2]
                    ngn = "ngc%d" % (it % 2)
                    kb.dma("sp", ngt, normg[n * 512:(n + 1) * 512].partition_broadcast(128), W=[ngn])
                    self.stt(tmp, self.psb(bank), 1.0, mbt, ALU.add, ALU.add, R=[self.PB(bank), mbn], W=["modtmp"])
                    self.tt("dve", o, tmp, ngt, ALU.mult, R=["modtmp", ngn], W=["modt"])
                else:
                    self.tt("dve", o, self.psb(bank), mbt, ALU.add, R=[self.PB(bank), mbn], W=["modt"])
                it += 1
        self.top = mark

    def norm_tile(self, xt, xname, A, sh, out, oname, sc):
        ss = sc["cols"][:, 0:1]
        r1 = sc["cols"][:, 1:2]
        r2 = sc["cols"][:, 2:3]
        rstd = sc["cols"][:, 3:4]
        self.act(sc["junk"], xt, AF.Square, R=[xname], W=["njunk", "ncols"], accum=ss)
        self.ts("dve", r1, ss, 1.0 / D, EPS, ALU.mult, ALU.add, R=["ncols"], W=["ncols"])
        self.act(r2, r1, AF.Sqrt, R=["ncols"], W=["ncols"])
        self.recip(rstd, r2, R=["ncols"], W=["ncols"])
        self.stt(sc["tmp"], xt, rstd, A, ALU.mult, ALU.mult, R=[xname, "ncols", "modt"], W=["ntmp"])
        self.tt("dve", out, sc["tmp"], sh, ALU.add, R=["ntmp", "modt"], W=[oname])

    def attention(self, l):
        kb = self.kb
        LAMINIT = 0.8 - 0.6 * math.exp(-0.3 * l)
        xin = self.dr["xloc"]
        w_in = self.dr["attn_w_in"]
        w_out = self.dr["attn_w_out"]
        self.top = self.CONST_TOP
        PH = self.top
        hT = self.alloc("hT", [DC, S], BF16)
        AFT_HT = self.top
        wqkv = self.alloc("wqkv", [DC, 768], BF16)
        otok = self.alloc("otok", [NT, D], BF16)
        small = self.top
        save = self.top
        self.top = 0
        qT = self.alloc("qT", [2, OWN], BF16)
        kT = self.alloc("kT", [2, S], BF16)
        V0 = self.alloc("V0", [NTF, 257], BF16)
        Vp = self.alloc("Vp", [NTF, 257], BF16)
        Vn = self.alloc("Vn", [NTF, 257], BF16)
        PT = [self.alloc("PT%d" % i, [512], BF16) for i in range(4)]
        biasT = self.alloc("biasT", [3, 8, 128], F32)
        ert = self.alloc("ert", [256], F32)
        self.o1buf = self.alloc("o1buf", [4, 256], F32)
        rr = self.alloc("rr", [512], F32)
        rr2 = self.alloc("rr2", [512], F32)
        assert self.top <= NT * D * 4, self.top
        self.top = save
        gq = self.alloc("gq", [1], F32)
        gk = self.alloc("gk", [1], F32)
        lamc = self.alloc("lamc", [8], F32)
        lv = self.alloc("lv", [4, 128], F32)
        sublg = self.alloc("sublg", [256], F32)
        sq = self.alloc("sq", [512], BF16)
        ocol = self.alloc("ocol", [8], F32)
        otmp = self.alloc("otmp", [256], F32)
        otmp2 = self.alloc("otmp2", [256], F32)
        ojunk = self.alloc("ojunk", [256], BF16)
        rt = self.alloc("rt", [8], F32)
        save2 = self.top
        self.top = AFT_HT
        A1 = self.alloc("A1", [D], F32)
        sh1 = self.alloc("sh1", [D], F32)
        xt = [self.alloc("xt%d" % i, [D], F32) for i in range(2)]
        hb = self.alloc("hb", [D], BF16)
        nsc = {"junk": self.alloc("njunk", [D], BF16), "tmp": self.alloc("ntmp", [D], F32),
               "cols": self.alloc("ncols", [4], F32)}
        self.mod_tiles(l, 0, A1, sh1, None, self.dr["norm1_g%d" % l], 0, nbuf=2)
        for t in range(NTF):
            x_t = xt[t % 2]
            xn = "xt%d" % (t % 2)
            kb.dma("sp", x_t, xin[t * 128:(t + 1) * 128, :], W=[xn])
            self.norm_tile(x_t, xn, A1, sh1, hb, "hb", nsc)
            for half in range(2):
                bank = 2 + half
                pb = self.psb(bank, BF16).rearrange("p (a b) -> p a b", b=128)
                for c in range(8):
                    dc = half * 8 + c
                    self.tr(pb[:, c, :], hb[:, dc * 128:(dc + 1) * 128], self.identb,
                            R=["hb", "c_identb"], W=[self.PB(bank)], inc=(c == 7))
                self.cp("act" if half == 0 else "dve", hT[:, half * 8:(half + 1) * 8, t * 128:(t + 1) * 128], pb,
                        R=[self.PB(bank)], W=["hT"])
        kb.barrier()
        self.top = save2
        kb.dma("sp", gq, self.dr["attn_q_gain"].rearrange("(p o) -> p o", o=1), W=["gq"])
        kb.dma("sp", gk, self.dr["attn_k_gain"].rearrange("(p o) -> p o", o=1), W=["gk"])
        self.ts("dve", gq, gq, 128.0 ** -0.5, None, ALU.mult, None, R=["gq"], W=["gq"])
        for i, nm in enumerate(["attn_lq1", "attn_lk1", "attn_lq2", "attn_lk2"]):
            kb.dma("sp", lv[:, i, :], self.dr[nm].partition_broadcast(128), W=["lv"])
        kb.dma("sp", sublg, self.dr["attn_subln_g"].partition_broadcast(128), W=["sublg"])
        self.ts("dve", sublg, sublg, 1.0 - LAMINIT, None, ALU.mult, None, R=["sublg"], W=["sublg"])
        self.tt("dve", lv[:, 0, :], lv[:, 0, :], lv[:, 1, :], ALU.mult, R=["lv"], W=["lv"])
        self.tt("dve", lv[:, 2, :], lv[:, 2, :], lv[:, 3, :], ALU.mult, R=["lv"], W=["lv"])
        kb.op("dve", lambda e: e.reduce_sum(lamc[:, 0:1], lv[:, 0, :], AX.X), R=["lv"], W=["lamc"])
        kb.op("dve", lambda e: e.reduce_sum(lamc[:, 1:2], lv[:, 2, :], AX.X), R=["lv"], W=["lamc"])
        self.act(lamc[:, 2:4], lamc[:, 0:2], AF.Exp, R=["lamc"], W=["lamc"])
        self.tt("dve", lamc[:, 4:5], lamc[:, 3:4], lamc[:, 2:3], ALU.subtract, R=["lamc"], W=["lamc"])
        self.ts("dve", lamc[:, 5:6], lamc[:, 4:5], -LAMINIT, None, ALU.add, None, R=["lamc"], W=["lamc"])
        neglam = lamc[:, 5:6]
        kb.dma("sp", ert[:, 0:16], self.dr["relfar"].rearrange("a b -> (a b)").partition_broadcast(128), W=["ert"])
        self.act(ert[:, 0:16], ert[:, 0:16], AF.Exp, R=["ert"], W=["ert"])
        ert3 = ert[:, 0:16].rearrange("p (u h) -> p u h", h=8)
        save3 = self.top
        self.top = AFT_HT + DC * 768 * 2
        bohs = self.alloc("bohs", [512], F32)
        Tsb = self.alloc("Tsb", [512], F32)
        self.top = save3
        kb.dma("sp", rt[0:32], self.dr["rel_table"], W=["rt"])
        kb.dma("sp", bohs[0:32, 0:511], self.dr["boh"], W=["bohs"])
        self.mm(self.psb(0)[0:8, 0:511], rt[0:32, :], bohs[0:32, 0:511], True, True, R=["rt", "bohs"], W=[self.PB(0)])
        self.cp("dve", Tsb[0:8, 0:511], self.psb(0)[0:8, 0:511], R=[self.PB(0)], W=["Tsb"])
        scr = self.nc.dram_tensor("biasscr", [8, 512], F32, kind="Internal").ap()
        kb.dma("sp", scr[:, 0:511], Tsb[0:8, 0:511], R=["Tsb"], W=["biasscr"])
        for di, dd in enumerate((-1, 0, 1)):
            for h in range(8):
                src = bass.AP(tensor=scr.tensor, offset=h * 512 + 128 - 128 * dd,
                              ap=[[1, 128], [1, 128]])
                kb.dma("sp", biasT[:, di, h, :], src, R=["biasscr"], W=["biasT"])
        kb.op("pool", lambda e: e.memset(V0[:, :, 256:257], 1.0), W=["V0ones"])
        for hh in range(8):
            for part, c0 in ((0, 2 * hh * 128), (1, D + 2 * hh * 128), (2, 2 * D + hh * 256)):
                kb.dma("pool", wqkv[:, :, part * 256:(part + 1) * 256],
                       w_in[:, c0:c0 + 256].rearrange("(k p) n -> p k n", p=128), W=["wqkv%d" % part])
            for isk in (0, 1):
                dst = kT if isk else qT
                dn = "kT" if isk else "qT"
                gcol = gk if isk else gq
                gn = "gk" if isk else "gq"
                for mi in range(2):
                    for tb in range(4 if isk else 2):
                        bank = (mi * 4 + tb) % 2
                        for dc in range(DC):
                            self.mm(self.psb(bank), wqkv[:, dc, isk * 256 + mi * 128: isk * 256 + (mi + 1) * 128],
                                    hT[:, dc, tb * 512:(tb + 1) * 512], dc == 0, dc == DC - 1,
                                    R=["wqkv%d" % isk, "hT"], W=[self.PB(bank)], inc=(dc == DC - 1))
                        self.act(sq, self.psb(bank), AF.Square, R=[self.PB(bank)], W=["sq"])
                        self.mm(self.psb(2 + bank), self.onesb, sq, True, True, R=["sq", "c_onesb"], W=[self.PB(2 + bank)])
                        self.ts("dve", rr, self.psb(2 + bank), 1.0 / 128, EPS, ALU.mult, ALU.add, R=[self.PB(2 + bank)], W=["rr"])
                        self.act(rr2, rr, AF.Sqrt, R=["rr"], W=["rr2"])
                        self.recip(rr, rr2, R=["rr2"], W=["rr"])
                        self.stt(dst[:, mi, tb * 512:(tb + 1) * 512], self.psb(bank), gcol, rr, ALU.mult, ALU.mult,
                                 R=[self.PB(bank), gn, "rr"], W=[dn])
            for i in range(NTF):
                bank = i % 2
                for dc in range(DC):
                    self.mm(self.psb(bank)[:, 0:256], hT[:, dc, i * 128:(i + 1) * 128], wqkv[:, dc, 512:768],
                            dc == 0, dc == DC - 1, R=["wqkv2", "hT"], W=[self.PB(bank)], inc=(dc == DC - 1))
                self.cp("act", V0[:, i, 0:256], self.psb(bank)[:, 0:256], R=[self.PB(bank)], W=["V0"])
            self.ts("dve", Vp, V0, ert3[:, 0, hh:hh + 1], None, ALU.mult, None, R=["V0", "V0ones", "ert"], W=["Vp"])
            self.ts("pool", Vn, V0, ert3[:, 1, hh:hh + 1], None, ALU.mult, None, R=["V0", "V0ones", "ert"], W=["Vn"])
            if hh == 0:
                self.dump("qT", qT, None, None)
            for qb in range(2):
                for mi in range(2):
                    def st_block(i):
                        bank = 2 + (i % 2)
                        near = [j for j in range(4 * qb, 4 * qb + 4) if abs(i - j) <= 1]
                        self.mm(self.psb(bank), kT[:, mi, i * 128:(i + 1) * 128], qT[:, mi, qb * 512:(qb + 1) * 512],
                                True, len(near) == 0, R=["kT", "qT"], W=[self.PB(bank)], inc=(len(near) == 0))
                        for n_i, j in enumerate(near):
                            jj = j - 4 * qb
                            self.mm(self.psb(bank)[:, jj * 128:(jj + 1) * 128], self.jrevf, biasT[:, (i - j) + 1, hh, :],
                                    False, n_i == len(near) - 1, R=["biasT", "c_jrevf"], W=[self.PB(bank)],
                                    inc=(n_i == len(near) - 1))
                        self.act(PT[i % 4], self.psb(bank), AF.Exp, R=[self.PB(bank)], W=["PT%d" % (i % 4)])

                    def pv_block(i):
                        for jj in range(4):
                            j = 4 * qb + jj
                            if i - j >= 2:
                                vv, vn = Vp, "Vp"
                            elif i - j <= -2:
                                vv, vn = Vn, "Vn"
                            else:
                                vv, vn = V0, "V0"
                            self.mm(self.psb(4 + jj)[:, 0:257], PT[i % 4][:, jj * 128:(jj + 1) * 128], vv[:, i, :],
                                    i == 0, i == NTF - 1, R=["PT%d" % (i % 4), vn, "V0ones"], W=[self.PB(4 + jj)],
                                    inc=(i == NTF - 1))
                    st_block(0)
                    for i in range(NTF):
                        if i + 1 < NTF:
                            st_block(i + 1)
                        pv_block(i)
                    for jj in range(4):
                        j = 4 * qb + jj
                        pso = self.psb(4 + jj)
                        cn = "ocol"
                        self.recip(ocol[:, 0:1], pso[:, 256:257], R=[self.PB(4 + jj)], W=[cn])
                        acc = otmp if jj % 2 == 0 else otmp2
                        an = "otmp%d" % (jj % 2)
                        if mi == 0:
                            self.ts("dve", self.o1buf[:, jj, :], pso[:, 0:256], ocol[:, 0:1], None, ALU.mult, None,
                                    R=[self.PB(4 + jj), cn], W=["o1buf%d" % jj])
                        else:
                            self.tt("dve", ocol[:, 1:2], ocol[:, 0:1], neglam, ALU.mult, R=[cn, "lamc"], W=[cn])
                            self.stt(acc, pso[:, 0:256], ocol[:, 1:2], self.o1buf[:, jj, :], ALU.mult, ALU.add,
                                     R=[self.PB(4 + jj), cn, "o1buf%d" % jj], W=[an])
                            self.act(ojunk, acc, AF.Square, R=[an], W=["ojunk", cn], accum=ocol[:, 2:3])
                            self.ts("dve", ocol[:, 3:4], ocol[:, 2:3], 1.0 / 256, EPS, ALU.mult, ALU.add, R=[cn], W=[cn])
                            self.act(ocol[:, 4:5], ocol[:, 3:4], AF.Sqrt, R=[cn], W=[cn])
                            self.recip(ocol[:, 5:6], ocol[:, 4:5], R=[cn], W=[cn])
                            self.stt(otok[:, j, hh * 256:(hh + 1) * 256], acc, ocol[:, 5:6], sublg, ALU.mult, ALU.mult,
                                     R=[an, cn, "sublg"], W=["otok"])
        kb.barrier()
        self.dump("otok", otok, None, None)
        self.top = PH
        wo = self.alloc("wo", [DC, D], BF16)
        self.top = AFT_HT
        g1 = self.alloc("g1", [D], F32)
        oT = self.alloc("oT", [DC, 128], BF16)
        xi = [self.alloc("xi%d" % i, [512], F32) for i in range(2)]
        xtmp = self.alloc("xtmp", [512], F32)
        assert self.top <= AFT_HT + DC * 768 * 2
        self.mod_tiles(l, 0, None, None, g1, None, 0, nbuf=2)
        kb.barrier()
        for n in range(4):
            kb.dma("pool", wo[:, :, n * 512:(n + 1) * 512],
                   w_out[:, n * 512:(n + 1) * 512].rearrange("(k p) n -> p k n", p=128), W=["wo%d" % n])
        Xv = self.X
        for j in range(NT):
            for half in range(2):
                bank = half
                pb = self.psb(bank, BF16).rearrange("p (a b) -> p a b", b=128)
                for c in range(8):
                    dc = half * 8 + c
                    self.tr(pb[:, c, :], otok[:, j, dc * 128:(dc + 1) * 128], self.identb,
                            R=["otok", "c_identb"], W=[self.PB(bank)], inc=(c == 7))
                self.cp("act", oT[:, half * 8:(half + 1) * 8, :], pb, R=[self.PB(bank)], W=["oT"])
            for n in range(4):
                bank = 2 + (n % 2)
                xit = xi[n % 2]
                xn = "xi%d" % (n % 2)
                kb.dma("sp", xit, xin[j * 128:(j + 1) * 128, n * 512:(n + 1) * 512], W=[xn])
                for c in range(DC):
                    self.mm(self.psb(bank), oT[:, c, :], wo[:, c, n * 512:(n + 1) * 512], c == 0, c == DC - 1,
                            R=["oT", "wo%d" % n], W=[self.PB(bank)], inc=(c == DC - 1))
                self.tt("dve", xtmp, self.psb(bank), g1[:, n * 512:(n + 1) * 512], ALU.mult,
                        R=[self.PB(bank), "modt"], W=["xtmp"])
                self.tt("dve", Xv[:, j, n * 512:(n + 1) * 512], xtmp, xit, ALU.add, R=["xtmp", xn], W=["X%d" % j])
        kb.barrier()


    def route_gather(self, l):
        kb = self.kb
        X = self.X
        self.top = self.MOE_BASE
        h2 = self.alloc("h2", [NT, D], BF16)
        g2 = self.alloc("g2", [D], F32)
        pos = self.alloc("pos", [NT, NE], F32)
        mask = self.alloc("mask", [NT, NE], F32)
        Gm = self.alloc("Gm", [NT, NE], F32)
        off_A2 = self.top
        A2 = self.alloc("A2", [D], F32)
        sh2 = self.alloc("sh2", [D], F32)
        h2f = self.alloc("h2f", [D], F32)
        h2T = self.alloc("h2T", [DC, 128], F32)
        nsc = {"junk": self.alloc("njunk", [D], BF16), "tmp": self.alloc("ntmp", [D], F32),
               "cols": self.alloc("ncols", [4], F32)}
        rw = self.alloc("rw", [DC, NE], F32)
        rb = self.alloc("rb", [NE], F32)
        logit = self.alloc("logit", [NT, NE], F32)
        top8 = self.alloc("top8", [NT, 8], F32)
        rcol = self.alloc("rcol", [NT, 4], F32)
        ex = self.alloc("ex", [NT, NE], F32)
        maskb = self.alloc("maskb", [NT, NE], BF16)
        posc = self.alloc("posc", [NT, NE], F32)
        self.mod_tiles(l, 1, A2, sh2, g2, self.dr["norm2_g%d" % l], self.top, nbuf=1)
        kb.barrier()
        pgT = self.alloc("pgT", [2, OWN], F32)
        kb.dma("sp", rw, self.dr["moe_router_w%d" % l].rearrange("(k p) e -> p k e", p=128), W=["rw"])
        kb.dma("sp", rb, self.dr["moe_router_b%d" % l].partition_broadcast(128), W=["rb"])
        for i in range(NT):
            self.norm_tile(X[:, i, :], "X%d" % i, A2, sh2, h2f, "h2f", nsc)
            self.cp("pool", h2[:, i, :], h2f, R=["h2f"], W=["h2"])
            for q4 in range(4):
                for c in range(4):
                    dc = q4 * 4 + c
                    self.tr(self.psb(q4)[:, c * 128:(c + 1) * 128], h2f[:, dc * 128:(dc + 1) * 128], self.identf,
                            R=["h2f", "c_identf"], W=[self.PB(q4)], inc=(c == 3))
                self.cp("act" if q4 % 2 else "dve", h2T[:, q4 * 4:(q4 + 1) * 4, :],
                        self.psb(q4).rearrange("p (a b) -> p a b", b=128), R=[self.PB(q4)], W=["h2T"])
            bank = 4 + (i % 2)
            for dc in range(DC):
                self.mm(self.psb(bank)[:, 0:NE], h2T[:, dc, :], rw[:, dc, :], dc == 0, dc == DC - 1,
                        R=["h2T", "rw"], W=[self.PB(bank)], inc=(dc == DC - 1))
            self.tt("dve", logit[:, i, :], self.psb(bank)[:, 0:NE], rb, ALU.add, R=[self.PB(bank), "rb"], W=["logit"])
        for i in range(NT):
            kb.op("dve", lambda e, i=i: e.max(top8[:, i, :], logit[:, i, :]), R=["logit"], W=["top8"])
            self.ts("dve", mask[:, i, :], logit[:, i, :], top8[:, i, 3:4], None, ALU.is_ge, None, R=["logit", "top8"], W=["mask"])
            self.ts("dve", rcol[:, i, 0:1], top8[:, i, 0:1], -1.0, None, ALU.mult, None, R=["top8"], W=["rcol"])
            self.act(ex[:, i, :], logit[:, i, :], AF.Exp, R=["logit", "rcol"], W=["ex"], bias=rcol[:, i, 0:1])
            self.tt("dve", ex[:, i, :], ex[:, i, :], mask[:, i, :], ALU.mult, R=["ex", "mask"], W=["ex"])
            kb.op("dve", lambda e, i=i: e.reduce_sum(rcol[:, i, 1:2], ex[:, i, :], AX.X), R=["ex"], W=["rcol"])
            self.recip(rcol[:, i, 2:3], rcol[:, i, 1:2], R=["rcol"], W=["rcol"])
            self.ts("dve", Gm[:, i, :], ex[:, i, :], rcol[:, i, 2:3], None, ALU.mult, None, R=["ex", "rcol"], W=["Gm"])
        self.cp("dve", maskb, mask, R=["mask"], W=["maskb"])
        for i in range(NT):
            bank = 4 + (i % 2)
            for j in range(i):
                self.mm(self.psb(bank)[:, 0:NE], self.onesb, maskb[:, j, :], j == 0, False,
                        R=["maskb", "c_onesb"], W=[self.PB(bank)], inc=False)
            self.mm(self.psb(bank)[:, 0:NE], self.trib, maskb[:, i, :], i == 0, True,
                    R=["maskb", "c_trib"], W=[self.PB(bank)])
            self.cp("dve", pos[:, i, :], self.psb(bank)[:, 0:NE], R=[self.PB(bank)], W=["pos"])
        self.ts("dve", posc, pos, 3000.0, None, ALU.min, None, R=["pos"], W=["posc"])
        for k, (nm, srcb, bank) in enumerate((("posc", posc, 6), ("Gm", Gm, 7))):
            for i in range(NT):
                half = i // 4
                self.tr(self.psb(bank - 2 * half)[0:32, (i % 4) * 128:(i % 4 + 1) * 128], srcb[:, i, :], self.identf,
                        R=[nm, "c_identf"], W=[self.PB(bank - 2 * half)], inc=(i % 4 == 3))
            for half in range(2):
                self.cp("dve", pgT[0:32, k, half * 512:(half + 1) * 512], self.psb(bank - 2 * half)[0:32, :],
                        R=[self.PB(bank - 2 * half)], W=["pgT"])
        pgo = self.dout("pgo", [2, NE, OWN])
        for k in range(2):
            kb.dma("sp", pgo[k], pgT[0:32, k, :], R=["pgT"], W=["pgo"])
        g2o = self.dout("g2o", [1, D])
        kb.dma("sp", g2o, g2[0:1, :], R=["modt"], W=["g2o"])
        kb.barrier()
        self.top = off_A2
        xTe = [self.alloc("xTe%d" % i, [DC, CAP], BF16) for i in range(2)]
        Se = [self.alloc("Se%d" % i, [NT, CAP], BF16) for i in range(2)]
        xg = self.nc.dram_tensor("xg", [NE, 128, DC * CAP], BF16, kind="ExternalOutput").ap()
        self.dr["xg"] = xg
        for e in range(NE):
            se = Se[e % 2]
            sn = "Se%d" % (e % 2)
            xt = xTe[e % 2]
            xn = "xTe%d" % (e % 2)
            for i in range(NT):
                self.ts("dve", se[:, i, :], self.iotarow, pos[:, i, e:e + 1], mask[:, i, e:e + 1], ALU.is_equal, ALU.mult,
                        R=["c_iotarow", "pos", "mask"], W=[sn])
            for dc in range(DC):
                bank = dc % 4
                for i in range(NT):
                    self.mm(self.psb(bank)[:, 0:CAP], h2[:, i, dc * 128:(dc + 1) * 128], se[:, i, :], i == 0, i == NT - 1,
                            R=[sn, "h2"], W=[self.PB(bank)], inc=(i == NT - 1))
                self.cp("act" if dc % 2 else "dve", xt[:, dc, :], self.psb(bank)[:, 0:CAP], R=[self.PB(bank)], W=[xn])
            kb.dma("sp", xg[e].rearrange("p (c s) -> p c s", s=CAP), xt, R=[xn], W=["xg"])
        kb.barrier()

    def ffn(self):
        kb = self.kb
        nc = self.nc
        NL = NE // 8
        xgin = nc.dram_tensor("xgin", [NL, 8, 128, DC * CAP], BF16, kind="ExternalInput").ap()
        self.dr["xgin"] = xgin
        w_gu = self.din("w_gu", [NL, D, 2 * D])
        bgu_d = self.din("bgu", [128, NL * 32])
        w_dn = self.din("w_dn", [NL, D, D])
        b_dn = self.din("b_dn", [NL, D])
        yo = nc.dram_tensor("yo", [NL, 8, CAP, D], BF16, kind="ExternalOutput").ap()
        self.dr["yo"] = yo
        self.top = 0
        SPP = 1024 // CAP
        HS = SPP * CAP
        xT = self.alloc("xT", [DC, HS], BF16)
        hdnT = self.alloc("hdnT", [DC, HS], BF16)
        wg = [self.alloc("wg%d" % i, [DC, 512], BF16) for i in range(2)]
        wd = [self.alloc("wd%d" % i, [DC, 512], BF16) for i in range(2)]
        bdt = [self.alloc("bdt%d" % i, [512], F32) for i in range(2)]
        ys = [self.alloc("ys%d" % i, [512], BF16) for i in range(2)]
        gc = [self.alloc("gc%d" % i, [512], F32) for i in range(2)]
        sg = [self.alloc("sg%d" % i, [512], F32) for i in range(2)]
        bgu = self.alloc("bgu", [NL, 32], F32)
        kb.dma("sp", bgu, bgu_d.rearrange("p (a b) -> p a b", b=32), W=["bgu"])
        ig = 0
        idn = 0
        iy = 0
        it = 0
        for el in range(NL):
            for half in range(8 // SPP):
                for s in range(SPP):
                    kb.dma("sp", xT[:, :, s * CAP:(s + 1) * CAP],
                           xgin[el, SPP * half + s].rearrange("p (c s) -> p c s", s=CAP), W=["xT"])
                for blk in range(8):
                    w = wg[ig % 2]
                    wn = "wg%d" % (ig % 2)
                    ig += 1
                    kb.dma("pool", w, w_gu[el, :, blk * 512:(blk + 1) * 512].rearrange("(k p) n -> p k n", p=128), W=[wn])
                    for c in range(4):
                        fc = blk * 4 + c
                        for sb in range(2):
                            bank = (fc % 4) * 2 + sb
                            for dc in range(DC):
                                self.mm(self.psb(bank), w[:, dc, c * 128:(c + 1) * 128], xT[:, dc, sb * 512:(sb + 1) * 512],
                                        dc == 0, dc == DC - 1, R=[wn, "xT"], W=[self.PB(bank)], inc=(dc == DC - 1))
                            g_ = gc[it % 2]
                            gn = "gc%d" % (it % 2)
                            s_ = sg[it % 2]
                            sn = "sg%d" % (it % 2)
                            it += 1
                            self.ts("dve", g_, self.psb(bank), bgu[:, el, fc:fc + 1], 7.0, ALU.add, ALU.min,
                                    R=[self.PB(bank), "bgu"], W=[gn])
                            if fc < 16:
                                self.act(s_, g_, AF.Sigmoid, R=[gn], W=[sn], scale=1.702)
                                self.tt("dve", hdnT[:, fc, sb * 512:(sb + 1) * 512], g_, s_, ALU.mult, R=[gn, sn], W=["hdnT"])
                            else:
                                j = fc - 16
                                self.ts("dve", g_, g_, -7.0, 1.0, ALU.max, ALU.add, R=[gn], W=[gn])
                                self.tt("dve", hdnT[:, j, sb * 512:(sb + 1) * 512], hdnT[:, j, sb * 512:(sb + 1) * 512], g_,
                                        ALU.mult, R=[gn, "hdnT"], W=["hdnT"])
                for n in range(4):
                    w = wd[idn % 2]
                    wn = "wd%d" % (idn % 2)
                    bt = bdt[idn % 2]
                    bn = "bdt%d" % (idn % 2)
                    idn += 1
                    kb.dma("pool", w, w_dn[el, :, n * 512:(n + 1) * 512].rearrange("(k p) n -> p k n", p=128), W=[wn])
                    kb.dma("sp", bt, b_dn[el, n * 512:(n + 1) * 512].partition_broadcast(128), W=[bn])
                    for st in range(8):
                        bank = st % 8
                        for fc in range(DC):
                            self.mm(self.psb(bank), hdnT[:, fc, st * 128:(st + 1) * 128], w[:, fc, :], fc == 0, fc == DC - 1,
                                    R=[wn, "hdnT"], W=[self.PB(bank)], inc=(fc == DC - 1))
                        y_ = ys[iy % 2]
                        yn = "ys%d" % (iy % 2)
                        iy += 1
                        self.tt("dve", y_, self.psb(bank), bt, ALU.add, R=[self.PB(bank), bn], W=[yn])
                        kb.dma("sp", yo[el, SPP * half + st // NCH, (st % NCH) * 128:(st % NCH + 1) * 128, n * 512:(n + 1) * 512], y_,
                               R=[yn], W=["yo"])
        kb.barrier()

    def scatter(self):
        kb = self.kb
        nc = self.nc
        xprev = self.din("xprev", [OWN, D])
        yin = nc.dram_tensor("yin", [NE, CAP, D], BF16, kind="ExternalInput").ap()
        self.dr["yin"] = yin
        pgi = self.din("pgi", [2, NE, OWN])
        g2i = self.din("g2i", [1, D])
        self.din("ident", [128, 128])
        self.din("iotap", [128, NCH])
        xo = self.dout("xo", [OWN, D])
        self.top = 0
        X = [self.alloc("Xt%d" % i, [D], F32) for i in range(2)]
        Fa = self.alloc("Fa", [NT, D], F32)
        g2 = self.alloc("g2", [D], F32)
        identf = self.alloc("identf", [128], F32)
        iotap = self.alloc("iotap", [NCH], F32)
        onesb = self.alloc("onesb", [128], F32)
        pgb = self.alloc("pgb", [2, OWN], F32)
        pm = self.alloc("pm", [2, OWN], F32)
        gb = self.alloc("gb", [2, 512], F32)
        SgT = [self.alloc("SgT%d" % i, [NCH, 2, 512], BF16) for i in range(2)]
        yw = [self.alloc("yw%d" % i, [NCH, D], BF16) for i in range(2)]
        tmp = self.alloc("stmp", [512], F32)
        kb.dma("sp", identf, self.dr["ident"], W=["identf"])
        kb.dma("sp", iotap, self.dr["iotap"], W=["iotap"])
        kb.dma("sp", g2, g2i[0].partition_broadcast(128), W=["g2"])
        kb.dma("sp", pgb[0:32], pgi.rearrange("k e t -> e k t"), W=["pgb"])
        kb.op("pool", lambda e: e.memset(onesb, 1.0), W=["onesb"])
        for e in range(NE):
            sgt = SgT[e % 2]
            sn = "SgT%d" % (e % 2)
            y_ = yw[e % 2]
            yn = "yw%d" % (e % 2)
            kb.dma("sp", y_, yin[e].rearrange("(c p) d -> p c d", p=128), W=[yn])
            for k in range(2):
                self.ts("dve", pm[0:32, k, :], pgb[0:32, k, :], identf[0:32, e:e + 1], None, ALU.mult, None,
                        R=["pgb", "identf"], W=["pm"])
            for hb in range(2):
                self.mm(self.psb(4 + hb), onesb[0:32, :], pm[0:32, 0, hb * 512:(hb + 1) * 512], True, True,
                        R=["pm", "onesb"], W=[self.PB(4 + hb)])
                self.mm(self.psb(6 + hb), onesb[0:32, :], pm[0:32, 1, hb * 512:(hb + 1) * 512], True, True,
                        R=["pm", "onesb"], W=[self.PB(6 + hb)])
            self.cp("act", gb, self.ps[:, 6:8, :], R=[self.PB(6), self.PB(7)], W=["gb"])
            for c in range(NCH):
                self.stt(sgt[:, c, :, :], self.ps[:, 4:6, :], iotap[:, c:c + 1], gb, ALU.is_equal, ALU.mult,
                         R=[self.PB(4), self.PB(5), "gb", "iotap"], W=[sn])
            for i in range(NT):
                for n in range(4):
                    bank = n
                    for c in range(NCH):
                        self.mm(self.psb(bank), sgt[:, c, :, :].rearrange("p a b -> p (a b)")[:, i * 128:(i + 1) * 128],
                                y_[:, c, n * 512:(n + 1) * 512], c == 0, c == NCH - 1, R=[sn, yn], W=[self.PB(bank)],
                                inc=(c == NCH - 1))
                    fsl = Fa[:, i, n * 512:(n + 1) * 512]
                    if e == 0:
                        self.cp("dve", fsl, self.psb(bank), R=[self.PB(bank)], W=["F%d" % i])
                    else:
                        self.tt("dve", fsl, self.psb(bank), fsl, ALU.add, R=[self.PB(bank), "F%d" % i], W=["F%d" % i])
        for i in range(NT):
            x_ = X[i % 2]
            xn = "Xt%d" % (i % 2)
            kb.dma("sp", x_, xprev[i * 128:(i + 1) * 128, :], W=[xn])
            for n in range(4):
                sl = slice(n * 512, (n + 1) * 512)
                self.tt("dve", tmp, Fa[:, i, sl], g2[:, sl], ALU.mult, R=["F%d" % i, "g2"], W=["stmp"])
                self.tt("dve", x_[:, sl], x_[:, sl], tmp, ALU.add, R=["stmp", xn], W=[xn])
            kb.dma("sp", xo[i * 128:(i + 1) * 128, :], x_, R=[xn], W=["xo"])
        kb.barrier()

    def rglru(self, l, xin):
        kb = self.kb
        w_in = self.dr["rec_w_in"]
        w_out = self.dr["rec_w_out"]
        self.top = self.CONST_TOP
        PH = self.top
        hT = self.alloc("hT", [DC, S], BF16)
        yT = self.alloc("yT", [DC, OWN], BF16)
        AFTER = self.top
        self.top = 44 * 1024
        xt = [self.alloc("xt%d" % i, [D], F32) for i in range(2)]
        self.top = AFTER
        A1 = self.alloc("A1", [D], F32)
        sh1 = self.alloc("sh1", [D], F32)
        hb = self.alloc("hb", [D], BF16)
        nsc = {"junk": self.alloc("njunk", [D], BF16), "tmp": self.alloc("ntmp", [D], F32),
               "cols": self.alloc("ncols", [4], F32)}
        self.mod_tiles(l, 0, A1, sh1, None, self.dr["norm1_g%d" % l], 0, nbuf=2)
        for t in range(NTF):
            x_t = xt[t % 2]
            xn = "xt%d" % (t % 2)
            kb.dma("sp", x_t, xin[t * 128:(t + 1) * 128, :], W=[xn])
            self.norm_tile(x_t, xn, A1, sh1, hb, "hb", nsc)
            for half in range(2):
                bank = 2 + half
                pb = self.psb(bank, BF16).rearrange("p (a b) -> p a b", b=128)
                for c in range(8):
                    dc = half * 8 + c
                    self.tr(pb[:, c, :], hb[:, dc * 128:(dc + 1) * 128], self.identb,
                            R=["hb", "c_identb"], W=[self.PB(bank)], inc=(c == 7))
                self.cp("act" if half == 0 else "dve", hT[:, half * 8:(half + 1) * 8, t * 128:(t + 1) * 128], pb,
                        R=[self.PB(bank)], W=["hT"])
        kb.barrier()
        self.top = 0
        Rb = self.alloc("Rb", [S], F32)
        Ib = self.alloc("Ib", [S], F32)
        Bb = self.alloc("Bb", [S], F32)
        Hb = self.alloc("Hb", [S], F32)
        ufp = self.alloc("ufp", [2, S], F32)
        ubf = self.alloc("ubf", [2, S], BF16)
        hsum = self.alloc("hsum", [2, OWN], F32)
        assert self.top <= NT * D * 4
        self.top = AFTER
        rec = self.alloc("rec", [S + 4], F32)
        wrec = self.alloc("wrec", [DC, 256], BF16)
        wgate = self.alloc("wgate", [DC, 256], BF16)
        wg4 = self.alloc("wg4", [4, 2, 256], BF16)
        c5 = self.alloc("c5", [DC, 5], F32)
        cb = self.alloc("cb", [DC], F32)
        gbias = self.alloc("gbias", [4, DC], F32)
        lamt = self.alloc("lamt", [2, DC], F32)
        nsp8 = self.alloc("nsp8", [2, DC], F32)
        nsp16 = self.alloc("nsp16", [2, DC], F32)
        gx2 = self.alloc("gx2", [512], F32)
        gin = self.alloc("gin", [512], F32)
        gsg = self.alloc("gsg", [512], F32)
        kb.dma("sp", c5, self.dr["rec_conv5"].rearrange("p (c j) -> p c j", j=5), W=["c5"])
        kb.dma("sp", cb, self.dr["rec_conv_b"], W=["cb"])
        for di in range(2):
            kb.dma("sp", gbias[:, 2 * di, :], self.dr["rec_ba"][di], W=["gbias"])
            kb.dma("sp", gbias[:, 2 * di + 1, :], self.dr["rec_bx"][di], W=["gbias"])
            kb.dma("sp", lamt[:, di, :], self.dr["rec_lam"][di], W=["lamt"])
        self.act(lamt, lamt, AF.Exp, R=["lamt"], W=["lamt"], scale=-1.0)
        self.act(lamt, lamt, AF.Ln, R=["lamt"], W=["lamt"], bias=1.0)
        self.ts("dve", nsp8, lamt, -8.0, None, ALU.mult, None, R=["lamt"], W=["nsp"])
        self.ts("dve", nsp16, lamt, -16.0, None, ALU.mult, None, R=["lamt"], W=["nsp"])
        kb.op("pool", lambda e: e.memset(rec, 0.0), W=["rec"])
        for n in range(8):
            kb.dma("pool", wrec, w_in[:, D + n * 256: D + (n + 1) * 256].rearrange("(k p) n -> p k n", p=128), W=["wrec"])
            kb.dma("pool", wgate, w_in[:, n * 256:(n + 1) * 256].rearrange("(k p) n -> p k n", p=128), W=["wgate"])
            for di in range(2):
                kb.dma("pool", wg4[:, 2 * di, :, :], self.dr["rec_wa"][di, n].rearrange("(k p) n -> p k n", p=128), W=["wg4"])
                kb.dma("pool", wg4[:, 2 * di + 1, :, :], self.dr["rec_wx"][di, n].rearrange("(k p) n -> p k n", p=128), W=["wg4"])
            for kc in range(2):
                cc = 2 * n + kc
                for tb in range(4):
                    for dc in range(DC):
                        self.mm(self.psb(tb), wrec[:, dc, kc * 128:(kc + 1) * 128], hT[:, dc, tb * 512:(tb + 1) * 512],
                                dc == 0, dc == DC - 1, R=["wrec", "hT"], W=[self.PB(tb)], inc=(dc == DC - 1))
                    self.cp("act", rec[:, 2 + tb * 512: 2 + (tb + 1) * 512], self.psb(tb), R=[self.PB(tb)], W=["rec"])
                un = "ufp%d" % kc
                self.ts("dve", ufp[:, kc, :], rec[:, 0:S], c5[:, cc, 0:1], cb[:, cc:cc + 1], ALU.mult, ALU.add,
                        R=["rec", "c5", "cb"], W=[un])
                for j in range(1, 5):
                    self.stt(ufp[:, kc, :], rec[:, j:j + S], c5[:, cc, j:j + 1], ufp[:, kc, :], ALU.mult, ALU.add,
                             R=["rec", "c5", un], W=[un])
                self.cp("pool", ubf[:, kc, :], ufp[:, kc, :], R=[un], W=["ubf%d" % kc])
            for di in range(2):
                T = OWN if di == 0 else S
                NB = T // 512
                for oc in range(2):
                    cc = 2 * n + oc
                    for gi, dstb, dn in ((0, Rb, "Rb"), (1, Ib, "Ib")):
                        for tb in range(NB):
                            bank = gi * 4 + tb
                            for kc in range(2):
                                self.mm(self.psb(bank), wg4[:, 2 * di + gi, kc, oc * 128:(oc + 1) * 128],
                                        ubf[:, kc, tb * 512:(tb + 1) * 512], kc == 0, kc == 1,
                                        R=["wg4", "ubf0", "ubf1"], W=[self.PB(bank)], inc=(kc == 1))
                            self.act(dstb[:, tb * 512:(tb + 1) * 512], self.psb(bank), AF.Sigmoid, R=[self.PB(bank), "gbias"],
                                     W=[dn], bias=gbias[:, 2 * di + gi, cc:cc + 1])
                    self.act(Bb[:, 0:T], Rb[:, 0:T], AF.Exp, R=["Rb", "nsp"], W=["Bb"], scale=nsp16[:, di, cc:cc + 1])
                    self.act(Rb[:, 0:T], Rb[:, 0:T], AF.Exp, R=["Rb", "nsp"], W=["Rb"], scale=nsp8[:, di, cc:cc + 1])
                    self.act(Bb[:, 0:T], Bb[:, 0:T], AF.Sqrt, R=["Bb"], W=["Bb"], scale=-1.0, bias=1.0)
                    self.tt("dve", Ib[:, 0:T], Ib[:, 0:T], ufp[:, oc, 0:T], ALU.mult, R=["Ib", "ufp%d" % oc], W=["Ib"])
                    self.tt("dve", Bb[:, 0:T], Bb[:, 0:T], Ib[:, 0:T], ALU.mult, R=["Bb", "Ib"], W=["Bb"])
                    if di == 0:
                        kb.op("dve", lambda e, oc=oc: e.tensor_tensor_scan(hsum[:, oc, :], Rb[:, 0:OWN], Bb[:, 0:OWN], 0.0,
                                                                          ALU.mult, ALU.add),
                              R=["Rb", "Bb"], W=["hsum%d" % oc])
                    else:
                        kb.op("dve", lambda e: e.tensor_tensor_scan(Hb[:, ::-1], Rb[:, ::-1], Bb[:, ::-1], 0.0,
                                                                    ALU.mult, ALU.add),
                              R=["Rb", "Bb"], W=["Hb"])
                        self.tt("dve", hsum[:, oc, :], hsum[:, oc, :], Hb[:, 0:OWN], ALU.add, R=["Hb", "hsum%d" % oc],
                                W=["hsum%d" % oc])
            for oc in range(2):
                cc = 2 * n + oc
                for tb in range(2):
                    bank = 2 * oc + tb
                    for dc in range(DC):
                        self.mm(self.psb(bank), wgate[:, dc, oc * 128:(oc + 1) * 128], hT[:, dc, tb * 512:(tb + 1) * 512],
                                dc == 0, dc == DC - 1, R=["wgate", "hT"], W=[self.PB(bank)], inc=(dc == DC - 1))
                    px = self.psb(bank)
                    self.act(gx2, px, AF.Square, R=[self.PB(bank)], W=["gx2"])
                    self.ts("dve", gin, gx2, 0.044715, 1.0, ALU.mult, ALU.add, R=["gx2"], W=["gin"])
                    self.tt("dve", gin, gin, px, ALU.mult, R=["gin", self.PB(bank)], W=["gin"])
                    self.act(gsg, gin, AF.Sigmoid, R=["gin"], W=["gsg"], scale=1.5957691216057308)
                    self.tt("dve", gsg, gsg, px, ALU.mult, R=["gsg", self.PB(bank)], W=["gsg"])
                    self.tt("dve", yT[:, cc, tb * 512:(tb + 1) * 512], gsg, hsum[:, oc, tb * 512:(tb + 1) * 512], ALU.mult,
                            R=["gsg", "hsum%d" % oc], W=["yT"])
        kb.barrier()
        self.dump("yT", yT, None, None)
        self.top = PH
        wo = self.alloc("wo", [DC, D], BF16)
        self.top = AFTER
        g1 = self.alloc("g1", [D], F32)
        xi = [self.alloc("xi%d" % i, [512], F32) for i in range(2)]
        xtmp = self.alloc("xtmp", [512], F32)
        self.mod_tiles(l, 0, None, None, g1, None, 0, nbuf=2)
        kb.barrier()
        for nn in range(4):
            kb.dma("pool", wo[:, :, nn * 512:(nn + 1) * 512],
                   w_out[:, nn * 512:(nn + 1) * 512].rearrange("(k p) n -> p k n", p=128), W=["wo%d" % nn])
        X = self.X
        for j in range(NT):
            for nn in range(4):
                bank = nn % 2
                xit = xi[nn % 2]
                xn = "xi%d" % (nn % 2)
                kb.dma("sp", xit, xin[j * 128:(j + 1) * 128, nn * 512:(nn + 1) * 512], W=[xn])
                for c in range(DC):
                    self.mm(self.psb(bank), yT[:, c, j * 128:(j + 1) * 128], wo[:, c, nn * 512:(nn + 1) * 512], c == 0, c == DC - 1,
                            R=["yT", "wo%d" % nn], W=[self.PB(bank)], inc=(c == DC - 1))
                self.tt("dve", xtmp, self.psb(bank), g1[:, nn * 512:(nn + 1) * 512], ALU.mult,
                        R=[self.PB(bank), "modt"], W=["xtmp"])
                self.tt("dve", X[:, j, nn * 512:(nn + 1) * 512], xtmp, xit, ALU.add, R=["xtmp", xn], W=["X%d" % j])
        kb.barrier()


LAYER_INPUTS = {
    "common": [("ident", [128, 128]), ("tri", [128, 128]), ("jrev", [128, 128]), ("iotarow", [128, CAP]), ("iotap", [128, NCH]),
               ("cbb", [128, DC * 128])],
    "moe": [("mod_w%d", [D, 6 * D]), ("mod_b%d", [6 * D]), ("norm1_g%d", [D]), ("norm2_g%d", [D]),
            ("moe_router_w%d", [D, NE]), ("moe_router_b%d", [NE]), ("moe_w_gu%d", [NE, D, 2 * D]),
            ("moe_b_gu%d", [NE, 2 * D]), ("moe_w_down%d", [NE, D, D]), ("moe_b_down%d", [NE, D])],
    "attn": [("attn_w_in", [D, 3 * D]), ("attn_q_gain", [128]), ("attn_k_gain", [128]), ("attn_lq1", [128]),
             ("attn_lk1", [128]), ("attn_lq2", [128]), ("attn_lk2", [128]), ("attn_subln_g", [256]),
             ("attn_w_out", [D, D]), ("rel_table", [32, 8]), ("relfar", [2, 8]), ("boh", [32, 511])],
    "rec": [("rec_w_in", [D, 2 * D]), ("rec_conv5", [128, DC * 5]), ("rec_conv_b", [128, DC]), ("rec_wa", [2, 8, 256, 256]),
            ("rec_ba", [2, 128, DC]), ("rec_wx", [2, 8, 256, 256]), ("rec_bx", [2, 128, DC]), ("rec_lam", [2, 128, DC]),
            ("rec_w_out", [D, D])],
}


def build(which, dumps=(), stop=None):
    p = Prog(which, dumps)
    kb = p.kb
    if which == "B":
        p.ffn()
        kb.emit()
        return p
    if which == "C":
        p.scatter()
        kb.emit()
        return p
    l = 0 if which == "A0" else 1
    p.din("xloc", [S, D])
    p.consts()
    for nm, shp in LAYER_INPUTS["moe"]:
        if nm.startswith("moe_w") or nm.startswith("moe_b"):
            continue
        if stop == "mixer" and (nm.startswith("moe") or nm.startswith("norm2")):
            continue
        p.din(nm % l, shp)
    if l == 0:
        for nm, shp in LAYER_INPUTS["attn"]:
            if nm not in p.dr:
                p.din(nm, shp)
        p.attention(0)
    else:
        for nm, shp in LAYER_INPUTS["rec"]:
            p.din(nm, shp)
        p.rglru(1, p.dr["xloc"])
    xo = p.dout("xo", [OWN, D])
    for j in range(NT):
        kb.dma("sp", xo[j * 128:(j + 1) * 128, :], p.X[:, j, :], R=["X%d" % j], W=["xo"])
    if stop != "mixer":
        p.route_gather(l)
    kb.barrier()
    kb.emit()
    return p


def _t5_bucket_np(rel):
    nb = 16
    max_exact = 8
    ret = np.where(rel > 0, nb, 0)
    n = np.abs(rel)
    nf = np.maximum(n, 1).astype(np.float32)
    large = max_exact + (np.log(nf / np.float32(max_exact)) / np.float32(math.log(128 / max_exact))
                         * np.float32(nb - max_exact)).astype(np.int32)
    large = np.minimum(large, nb - 1)
    return ret + np.where(n < max_exact, n, large)


def host_consts():
    c = {}
    c["ident"] = np.eye(128, dtype=np.float32)
    c["jrev"] = np.ascontiguousarray(np.eye(128, dtype=np.float32)[::-1])
    c["tri"] = np.triu(np.ones((128, 128), np.float32), k=1)
    c["iotarow"] = np.tile(np.arange(CAP, dtype=np.float32)[None, :], (128, 1))
    c["iotap"] = (np.arange(128, dtype=np.float32)[:, None] + 128.0 * np.arange(NCH, dtype=np.float32)[None, :])
    return c


def core_inputs(which, core, inp, xfull):
    b, hf = core // 2, core % 2
    l = 0 if which == "A0" else 1
    m = dict(host_consts())
    xb = xfull[b]
    m["xloc"] = np.ascontiguousarray(xb if hf == 0 else xb[::-1])
    cb = inp["c"][b].reshape(DC, 128).T
    m["cbb"] = np.ascontiguousarray(np.repeat(cb[:, :, None], 128, axis=2).reshape(128, DC * 128))
    for nm, _ in LAYER_INPUTS["moe"]:
        if nm[:-2] in inp and not (nm.startswith("moe_w") or nm.startswith("moe_b")):
            m[nm % l] = inp[nm[:-2]][l]
    if l == 0:
        for nm in ("attn_w_in", "attn_q_gain", "attn_k_gain", "attn_lq1", "attn_lk1", "attn_lq2", "attn_lk2",
                   "attn_subln_g", "attn_w_out"):
            m[nm] = inp[nm][0]
        m["rel_table"] = inp["rel_table"]
        sgn = 1 if hf == 0 else -1
        rloc = 255 - np.arange(511)
        bk = _t5_bucket_np((sgn * rloc).astype(np.int32))
        boh = np.zeros((32, 511), np.float32)
        boh[bk, np.arange(511)] = 1.0
        m["boh"] = boh
        rows = [31, 15] if hf == 0 else [15, 31]
        m["relfar"] = np.ascontiguousarray(inp["rel_table"][rows])
    else:
        m["rec_w_in"] = inp["rec_w_in"][0]
        cw = inp["rec_conv_w"][0]
        c5 = np.zeros((D, 5), np.float32)
        if hf == 0:
            c5[:, 0:4] = cw.T
        else:
            c5[:, 1:5] = cw[::-1].T
        m["rec_conv5"] = np.ascontiguousarray(c5.reshape(DC, 128, 5).transpose(1, 0, 2).reshape(128, DC * 5))
        m["rec_conv_b"] = np.ascontiguousarray(inp["rec_conv_b"][0].reshape(DC, 128).T)
        dirs = [0, 1] if hf == 0 else [1, 0]
        for nm in ("rec_wa", "rec_wx"):
            m[nm] = np.ascontiguousarray(inp[nm][0][dirs])
        for nm in ("rec_ba", "rec_bx", "rec_lam"):
            m[nm] = np.ascontiguousarray(inp[nm][0][dirs].reshape(2, DC, 128).transpose(0, 2, 1))
        m["rec_w_out"] = inp["rec_w_out"][0]
    return m


_PROGS = {}


def _prog(which, **kw):
    if which not in _PROGS:
        _PROGS[which] = build(which, **kw)
    return _PROGS[which]


def _launch(p, in_maps):
    maps = []
    for cm in in_maps:
        mm = {}
        for k in p.dr:
            if k in cm:
                a = cm[k]
                mm[k] = np.ascontiguousarray(a) if a.dtype != np.float64 else np.ascontiguousarray(a, dtype=np.float32)
        maps.append(mm)
    return run_bass_kernel_spmd(p.nc, maps, core_ids=list(range(8)))


def run_A(l, inp, xfull, prog=None):
    which = "A%d" % l
    p = prog or _prog(which)
    res = _launch(p, [core_inputs(which, core, inp, xfull) for core in range(8)])
    return res.results


def run_B(l, inp, resA, prog=None):
    p = prog or _prog("B")
    NL = NE // 8
    in_maps = []
    for r in range(8):
        m = {}
        m["xgin"] = np.ascontiguousarray(
            np.stack([np.stack([resA[s]["xg"][NL * r + el] for s in range(8)]) for el in range(NL)]))
        m["w_gu"] = inp["moe_w_gu"][l][NL * r:NL * (r + 1)]
        bg = inp["moe_b_gu"][l][NL * r:NL * (r + 1)]
        m["bgu"] = np.ascontiguousarray(bg.reshape(NL, 32, 128).transpose(2, 0, 1).reshape(128, NL * 32))
        m["w_dn"] = inp["moe_w_down"][l][NL * r:NL * (r + 1)]
        m["b_dn"] = np.ascontiguousarray(inp["moe_b_down"][l][NL * r:NL * (r + 1)])
        in_maps.append(m)
    return _launch(p, in_maps).results


def run_C(resA, resB, prog=None):
    p = prog or _prog("C")
    NL = NE // 8
    hc = host_consts()
    in_maps = []
    for s in range(8):
        m = {"ident": hc["ident"], "iotap": hc["iotap"]}
        m["xprev"] = resA[s]["xo"]
        m["pgi"] = resA[s]["pgo"]
        m["g2i"] = resA[s]["g2o"]
        m["yin"] = np.ascontiguousarray(np.stack([resB[e // NL]["yo"][e % NL, s] for e in range(NE)]))
        in_maps.append(m)
    return _launch(p, in_maps).results


def assemble(res, key="xo"):
    out = np.empty((4, S, D), np.float32)
    for core in range(8):
        b, hf = core // 2, core % 2
        xo = np.asarray(res[core][key], dtype=np.float32)
        if hf == 0:
            out[b, 0:OWN] = xo
        else:
            out[b, OWN:S] = xo[::-1]
    return out


def kernel(**inputs):
    inp = {k: np.asarray(v) for k, v in inputs.items()}
    x = np.asarray(inp["x"], dtype=np.float32)
    for l in range(2):
        resA = run_A(l, inp, x)
        resB = run_B(l, inp, resA)
        resC = run_C(resA, resB)
        x = assemble(resC)
    return x
```
